# Optimizing a Trainium2 kernel written in Bass

```python
import functools
import jax, jax.numpy as jnp
from jax import lax
import numpy as np

D_MODEL = 2048
BATCH = 16
SEQ = 256
DEPTH = 2
DEC_BATCH = 4
DEC_SEQ = 4096
PAST_LEN = 256

GRID_W = 64
EPS = 1e-6
MIX_W = D_MODEL
CONV_C = 512
CONV_K = 31
RET_H = 4
RET_DK = 128
RET_DV = 128
RET_W = RET_H * RET_DV
RET_CHUNK = 128
RET_EXP_MIN = 5.0
RET_EXP_MAX = 12.0
MLA_H = 8
MLA_DN = 128
MLA_DR = 64
MLA_DV = 128
MLA_Q_LORA = 768
MLA_KV_LORA = 256
MLA_W = MLA_H * MLA_DV
MLA_SCALE = (MLA_DN + MLA_DR) ** -0.5
ROPE_BASE = 10000.0
ROPE_AXIS = MLA_DR // 2
Q_BLOCK = 128
IN_SIZES = (2 * CONV_C, RET_H * RET_DK, RET_H * RET_DK, RET_H * RET_DV, RET_W, MLA_Q_LORA, MLA_KV_LORA, MLA_DR)
IN_COLS = sum(IN_SIZES)
D_FF = 5632
N_EXPERTS = 8
TOP_K = 2
D_EXPERT = 7168
MOE_BLOCK = 128
N_DENSE = (DEPTH + 1) // 2
N_MOE = DEPTH // 2

kernel_name = 'hymba_conv_retnet_mla_diffusion_step'


def rms_norm(x, g):
    xf = x.astype(jnp.float32)
    y = xf * lax.rsqrt(jnp.mean(xf * xf, axis=-1, keepdims=True) + EPS)
    return (y * g.astype(jnp.float32)).astype(x.dtype)


def layer_norm(x, g, b):
    xf = x.astype(jnp.float32)
    mu = jnp.mean(xf, axis=-1, keepdims=True)
    var = jnp.mean(jnp.square(xf - mu), axis=-1, keepdims=True)
    y = (xf - mu) * lax.rsqrt(var + EPS)
    return (y * g.astype(jnp.float32) + b.astype(jnp.float32)).astype(x.dtype)


def head_norm(o, g):
    of = o.astype(jnp.float32)
    mu = jnp.mean(of, axis=-1, keepdims=True)
    var = jnp.mean(jnp.square(of - mu), axis=-1, keepdims=True)
    y = ((of - mu) * lax.rsqrt(var + EPS)).reshape(o.shape[:-2] + (-1,))
    return (y * g.astype(jnp.float32)).astype(o.dtype)


def split_heads(a, n_heads):
    return a.reshape(a.shape[:-1] + (n_heads, a.shape[-1] // n_heads))


def grid_axial_angles(n_tokens):
    rows = n_tokens // GRID_W
    row = jnp.repeat(jnp.arange(rows), GRID_W).astype(jnp.float32)
    col = (jnp.arange(n_tokens) % GRID_W).astype(jnp.float32)
    inv_freq = jnp.power(ROPE_BASE, -jnp.arange(0, ROPE_AXIS, 2, dtype=jnp.float32) / ROPE_AXIS)
    return row[:, None] * inv_freq, col[:, None] * inv_freq


def rotate_pairs(y, ang):
    y1, y2 = jnp.split(y, 2, axis=-1)
    cos, sin = jnp.cos(ang), jnp.sin(ang)
    return jnp.concatenate([y1 * cos - y2 * sin, y2 * cos + y1 * sin], axis=-1)


def axial_rope(x, ang_row, ang_col):
    x_row, x_col = jnp.split(x.astype(jnp.float32), 2, axis=-1)
    return jnp.concatenate([rotate_pairs(x_row, ang_row), rotate_pairs(x_col, ang_col)], axis=-1).astype(x.dtype)


def conformer_conv(z, dw_w, dw_b, ln_g, ln_b):
    a, gate = jnp.split(z, 2, axis=-1)
    u = a * jax.nn.sigmoid(gate)
    u = lax.conv_general_dilated(u, dw_w[:, None, :], window_strides=(1,),
                                 padding=[(CONV_K // 2, CONV_K // 2)],
                                 dimension_numbers=('NWC', 'WIO', 'NWC'),
                                 feature_group_count=CONV_C) + dw_b
    return jax.nn.silu(layer_norm(u, ln_g, ln_b))


def retention_scan(q, k, v, log_gamma, s0):
    B, T, H, _ = q.shape
    dv = v.shape[-1]
    n = T // RET_CHUNK

    def chunks(a):
        return a.astype(jnp.float32).reshape(B, n, RET_CHUNK, H, a.shape[-1]).transpose(1, 0, 3, 2, 4)

    idx = jnp.arange(RET_CHUNK, dtype=jnp.float32)
    diff = idx[:, None] - idx[None, :]
    lg = log_gamma.astype(jnp.float32)
    decay = jnp.where(diff >= 0, jnp.exp(jnp.maximum(diff, 0.0) * lg[:, None, None]), 0.0)
    q_decay = jnp.exp((idx + 1.0) * lg[:, None])[..., None]
    k_decay = jnp.exp((RET_CHUNK - 1.0 - idx) * lg[:, None])[..., None]
    chunk_decay = jnp.exp(RET_CHUNK * lg)[:, None, None]

    def step(s, qkv):
        qc, kc, vc = qkv
        att = jnp.einsum('bhid,bhjd->bhij', qc, kc) * decay
        o = jnp.einsum('bhij,bhjv->bhiv', att, vc) + q_decay * jnp.einsum('bhid,bhdv->bhiv', qc, s)
        s = chunk_decay * s + jnp.einsum('bhjd,bhjv->bhdv', kc * k_decay, vc)
        return s, o

    s, o = lax.scan(step, s0.astype(jnp.float32), (chunks(q), chunks(k), chunks(v)))
    o = o.transpose(1, 0, 3, 2, 4).reshape(B, T, H, dv)
    return o.astype(q.dtype), s


def block_attention(q_nope, q_rope, k_nope, k_rope, v):
    B, Tq, H, _ = q_nope.shape
    nb = Tq // Q_BLOCK

    def blocks(a):
        return jnp.moveaxis(a.reshape((B, nb, Q_BLOCK) + a.shape[2:]), 1, 0)

    def one_block(qs):
        qn, qr = qs
        s = jnp.einsum('bqhd,bkhd->bhqk', qn, k_nope) + jnp.einsum('bqhr,bkr->bhqk', qr, k_rope)
        p = jax.nn.softmax(s.astype(jnp.float32) * MLA_SCALE, axis=-1).astype(v.dtype)
        return jnp.einsum('bhqk,bkhd->bqhd', p, v)

    o = lax.map(one_block, (blocks(q_nope), blocks(q_rope)))
    return jnp.moveaxis(o, 0, 1).reshape(B, Tq, H * v.shape[-1])


def dense_swiglu(h, w_gate, w_up, w_down):
    return (jax.nn.silu(h @ w_gate) * (h @ w_up)) @ w_down


def moe_swiglu(h, w_router, b_router, w_gate, w_up, w_down):
    lead = h.shape[:-1]
    x = h.reshape(-1, h.shape[-1])
    n_tok = x.shape[0]
    n_assign = n_tok * TOP_K
    n_blocks = -(-n_assign // MOE_BLOCK) + N_EXPERTS
    n_slots = n_blocks * MOE_BLOCK
    logits = (x @ w_router).astype(jnp.float32) + b_router.astype(jnp.float32)
    top_logit, top_idx = lax.top_k(logits, TOP_K)
    gates = jax.nn.softmax(top_logit, axis=-1).reshape(-1)
    expert = top_idx.reshape(-1)
    token = jnp.repeat(jnp.arange(n_tok, dtype=jnp.int32), TOP_K)
    order = jnp.argsort(expert)
    e_sorted = expert[order]
    counts = jnp.bincount(expert, length=N_EXPERTS)
    start = jnp.cumsum(counts) - counts
    padded = (counts + MOE_BLOCK - 1) // MOE_BLOCK * MOE_BLOCK
    padded_end = jnp.cumsum(padded)
    padded_start = padded_end - padded
    dest = padded_start[e_sorted] + jnp.arange(n_assign) - start[e_sorted]
    slot_token = jnp.full((n_slots,), n_tok, jnp.int32).at[dest].set(token[order])
    slot_gate = jnp.zeros((n_slots,), jnp.float32).at[dest].set(gates[order])
    block_expert = jnp.minimum(
        jnp.searchsorted(padded_end, jnp.arange(n_blocks) * MOE_BLOCK, side='right'), N_EXPERTS - 1)
    x_pad = jnp.concatenate([x, jnp.zeros((1, x.shape[-1]), x.dtype)], axis=0)
    xs = x_pad[slot_token].reshape(n_blocks, MOE_BLOCK, -1)

    def expert_block(args):
        xb, e = args
        return (jax.nn.silu(xb @ w_gate[e]) * (xb @ w_up[e])) @ w_down[e]

    ys = lax.map(expert_block, (xs, block_expert)).reshape(n_slots, -1)
    out = jax.ops.segment_sum(ys * slot_gate[:, None].astype(ys.dtype), slot_token, num_segments=n_tok + 1)
    return out[:n_tok].reshape(lead + (-1,))


def trunk_layer(x, cond, lw, channel_mixer, ctx):
    B, T, _ = x.shape
    mod = (jax.nn.silu(cond) @ lw['w_mod'] + lw['b_mod'])[..., None, :]
    sh1, sc1, g1, sh2, sc2, g2 = jnp.split(mod, 6, axis=-1)
    h = rms_norm(x, lw['norm'][0]) * (1.0 + sc1) + sh1
    cuts = [int(i) for i in np.cumsum(IN_SIZES)[:-1]]
    conv_in, rq, rk, rv, rg, cq, ckv, kr = jnp.split(h @ lw['w_in'], cuts, axis=-1)

    y_conv = conformer_conv(conv_in, lw['conv_w'], lw['conv_b'], lw['conv_ln_g'], lw['conv_ln_b'])

    q = split_heads(rq, RET_H)
    k = split_heads(rk, RET_H) * (RET_DK ** -0.5)
    v = split_heads(rv, RET_H)
    log_gamma = jax.nn.log_sigmoid(lw['ret_decay'].astype(jnp.float32))
    if ctx is None:
        s0_f = jnp.zeros((B, RET_H, RET_DK, RET_DV), jnp.float32)
        s0_b = s0_f
    else:
        s0_f, s0_b = ctx[2][:, 0], ctx[2][:, 1]
    o_f, s_f = retention_scan(q, k, v, log_gamma[0], s0_f)
    o_b, s_b = retention_scan(q[:, ::-1], k[:, ::-1], v[:, ::-1], log_gamma[1], s0_b)
    y_ret = head_norm(o_f + o_b[:, ::-1], lw['ret_gn']) * jax.nn.silu(rg)

    qa = split_heads(rms_norm(cq, lw['q_norm']) @ lw['w_uq'], MLA_H)
    q_nope, q_rope = qa[..., :MLA_DN], qa[..., MLA_DN:]
    ckv_n = rms_norm(ckv, lw['kv_norm'])
    kv = split_heads(ckv_n @ lw['w_ukv'], MLA_H)
    k_nope, v_mla = kv[..., :MLA_DN], kv[..., MLA_DN:]
    if ctx is None:
        y_mla = block_attention(q_nope, q_rope, k_nope, kr, v_mla)
        new_state = (ckv_n, kr, jnp.stack([s_f, s_b], axis=1))
    else:
        ckv_ctx, kpe_ctx, _ = ctx
        ang_row, ang_col = grid_axial_angles(T)
        q_rope = axial_rope(q_rope, ang_row[:, None, :], ang_col[:, None, :])
        kr_lat = axial_rope(kr, ang_row, ang_col)
        kv_ctx = split_heads(ckv_ctx @ lw['w_ukv'], MLA_H)
        k_all = jnp.concatenate([kv_ctx[..., :MLA_DN], k_nope], axis=1)
        kr_all = jnp.concatenate([kpe_ctx, kr_lat], axis=1)
        v_all = jnp.concatenate([kv_ctx[..., MLA_DN:], v_mla], axis=1)
        y_mla = block_attention(q_nope, q_rope, k_all, kr_all, v_all)
        new_state = None

    y = jnp.concatenate([y_conv, y_ret, y_mla], axis=-1) @ lw['w_out']
    x = x + g1 * rms_norm(y, lw['norm'][1])
    h = rms_norm(x, lw['norm'][2]) * (1.0 + sc2) + sh2
    x = x + g2 * rms_norm(channel_mixer(h), lw['norm'][3])
    return x, new_state


def setup_inputs(seed: int = 0) -> dict:
    key = jax.random.key(seed)
    k = jax.random.split(key, 30)
    f32 = jnp.float32
    D = D_MODEL

    def nrm(i, shape, scale=1.0):
        return jax.random.normal(k[i], shape, f32) * scale

    def gain(i, shape):
        return 1.0 + nrm(i, shape, 0.05)

    decay_base = jnp.log(jnp.exp2(jnp.linspace(RET_EXP_MIN, RET_EXP_MAX, RET_H)) - 1.0)
    return {
        'x_prompt': nrm(0, (BATCH, SEQ, D)),
        'x_sample': nrm(1, (DEC_BATCH, DEC_SEQ, D)),
        'cache_mla_ckv': nrm(2, (DEC_BATCH, DEPTH, PAST_LEN, MLA_KV_LORA)),
        'cache_mla_kpe': nrm(3, (DEC_BATCH, DEPTH, PAST_LEN, MLA_DR)),
        'state_ret': nrm(4, (DEC_BATCH, DEPTH, 2, RET_H, RET_DK, RET_DV)),
        'c': nrm(5, (DEC_BATCH, D)),
        'c_ctx': nrm(6, (D,)),
        'w_mod': nrm(7, (DEPTH, D, 6 * D), 0.5 * D ** -0.5),
        'b_mod': nrm(8, (DEPTH, 6 * D), 0.02),
        'norm_gains': gain(9, (DEPTH, 4, D)),
        'w_in': nrm(10, (DEPTH, D, IN_COLS), D ** -0.5),
        'w_out': nrm(11, (DEPTH, MIX_W, D), MIX_W ** -0.5),
        'conv_w': nrm(12, (DEPTH, CONV_K, CONV_C), CONV_K ** -0.5),
        'conv_b': nrm(13, (DEPTH, CONV_C), 0.02),
        'conv_ln_g': gain(14, (DEPTH, CONV_C)),
        'conv_ln_b': nrm(15, (DEPTH, CONV_C), 0.02),
        'ret_decay_logit': decay_base + nrm(16, (DEPTH, 2, RET_H), 0.1),
        'ret_gn_g': gain(17, (DEPTH, RET_W)),
        'mla_q_norm': gain(18, (DEPTH, MLA_Q_LORA)),
        'mla_w_uq': nrm(19, (DEPTH, MLA_Q_LORA, MLA_H * (MLA_DN + MLA_DR)), MLA_Q_LORA ** -0.5),
        'mla_kv_norm': gain(20, (DEPTH, MLA_KV_LORA)),
        'mla_w_ukv': nrm(21, (DEPTH, MLA_KV_LORA, MLA_H * (MLA_DN + MLA_DV)), MLA_KV_LORA ** -0.5),
        'ffn_w_gate': nrm(22, (N_DENSE, D, D_FF), D ** -0.5),
        'ffn_w_up': nrm(23, (N_DENSE, D, D_FF), D ** -0.5),
        'ffn_w_down': nrm(24, (N_DENSE, D_FF, D), D_FF ** -0.5),
        'moe_w_router': nrm(25, (N_MOE, D, N_EXPERTS), D ** -0.5),
        'moe_b_router': nrm(26, (N_MOE, N_EXPERTS), 0.01),
        'moe_w_gate': nrm(27, (N_MOE, N_EXPERTS, D, D_EXPERT), D ** -0.5),
        'moe_w_up': nrm(28, (N_MOE, N_EXPERTS, D, D_EXPERT), D ** -0.5),
        'moe_w_down': nrm(29, (N_MOE, N_EXPERTS, D_EXPERT, D), D_EXPERT ** -0.5),
    }


def reference(x_prompt, x_sample, cache_mla_ckv, cache_mla_kpe, state_ret, c, c_ctx,
              w_mod, b_mod, norm_gains, w_in, w_out, conv_w, conv_b, conv_ln_g, conv_ln_b,
              ret_decay_logit, ret_gn_g, mla_q_norm, mla_w_uq, mla_kv_norm, mla_w_ukv,
              ffn_w_gate, ffn_w_up, ffn_w_down,
              moe_w_router, moe_b_router, moe_w_gate, moe_w_up, moe_w_down):
    def layer_weights(l):
        return {'w_mod': w_mod[l], 'b_mod': b_mod[l], 'norm': norm_gains[l],
                'w_in': w_in[l], 'w_out': w_out[l],
                'conv_w': conv_w[l], 'conv_b': conv_b[l], 'conv_ln_g': conv_ln_g[l], 'conv_ln_b': conv_ln_b[l],
                'ret_decay': ret_decay_logit[l], 'ret_gn': ret_gn_g[l],
                'q_norm': mla_q_norm[l], 'w_uq': mla_w_uq[l], 'kv_norm': mla_kv_norm[l], 'w_ukv': mla_w_ukv[l]}

    def channel_mixer(l):
        i = l // 2
        if l % 2 == 0:
            return functools.partial(dense_swiglu, w_gate=ffn_w_gate[i], w_up=ffn_w_up[i], w_down=ffn_w_down[i])
        return functools.partial(moe_swiglu, w_router=moe_w_router[i], b_router=moe_b_router[i],
                                 w_gate=moe_w_gate[i], w_up=moe_w_up[i], w_down=moe_w_down[i])

    y_prompt = x_prompt
    ckv_layers, kpe_layers, ret_layers = [], [], []
    for l in range(DEPTH):
        y_prompt, (ckv_l, kpe_l, ret_l) = trunk_layer(y_prompt, c_ctx, layer_weights(l), channel_mixer(l), None)
        ckv_layers.append(ckv_l)
        kpe_layers.append(kpe_l)
        ret_layers.append(ret_l)
    new_mla_ckv = jnp.stack(ckv_layers, axis=1)
    new_mla_kpe = jnp.stack(kpe_layers, axis=1)
    new_state_ret = jnp.stack(ret_layers, axis=1)

    y_sample = x_sample
    for l in range(DEPTH):
        ctx = (cache_mla_ckv[:, l], cache_mla_kpe[:, l], state_ret[:, l])
        y_sample, _ = trunk_layer(y_sample, c, layer_weights(l), channel_mixer(l), ctx)

    return (y_prompt, y_sample, new_mla_ckv, new_mla_kpe, new_state_ret)
```

```python
import contextlib
import numpy as np
import concourse.bass as bass
import concourse.mybir as mybir
from concourse.bass_utils import run_bass_kernel_spmd

F32 = mybir.dt.float32
BF16 = mybir.dt.bfloat16
AF = mybir.ActivationFunctionType
ALU = mybir.AluOpType
AX = mybir.AxisListType

PE, ACT, DVE, POOL, SP = "pe", "act", "dve", "pool", "sp"
COMPUTE = (PE, ACT, DVE, POOL)
ALL_ENG = (PE, ACT, DVE, POOL, SP)


class Res:
    __slots__ = ("name", "cw", "dw", "cr", "dr")

    def __init__(self, name=""):
        self.name = name
        self.cw = {}
        self.dw = []
        self.cr = {}
        self.dr = []


class NoTrack(Res):
    __slots__ = ()


class Op:
    __slots__ = ("eng", "emit", "deps", "is_dma", "signal", "sem", "val", "prewait")

    def __init__(self, eng, emit, is_dma):
        self.eng = eng
        self.emit = emit
        self.deps = []
        self.is_dma = is_dma
        self.signal = False
        self.sem = None
        self.val = None
        self.prewait = None


class Sched:
    def __init__(self, nc):
        self.nc = nc
        self.streams = {e: [] for e in ALL_ENG}
        self.n_dma_sems = {SP: 24, POOL: 24, ACT: 8}
        self.pending_dma = []
        self.last_c = {}
        self.stopped = False

    def _add(self, eng, emit, reads, writes, is_dma):
        op = Op(eng, emit, is_dma)
        if self.stopped:
            return op
        reads = [r for r in reads if not isinstance(r, NoTrack)]
        writes = [r for r in writes if not isinstance(r, NoTrack)]
        deps = []
        for r in reads:
            deps.extend(r.cw.values())
            deps.extend(r.dw)
        for r in writes:
            for e, w in r.cw.items():
                if is_dma or e != eng or eng != PE:
                    deps.append(w)
            deps.extend(r.dw)
            for e, w in r.cr.items():
                if is_dma or e != eng or eng != PE:
                    deps.append(w)
            deps.extend(r.dr)
        seen = set()
        for d in deps:
            if id(d) not in seen:
                seen.add(id(d))
                op.deps.append(d)
                d.signal = True
        for r in writes:
            if is_dma:
                r.dw = [op]
                r.cw = {}
            else:
                r.cw = {eng: op}
                r.dw = []
            r.cr = {}
            r.dr = []
        for r in reads:
            if r in writes:
                continue
            if is_dma:
                r.dr.append(op)
            else:
                r.cr[eng] = op
        self.streams[eng].append(op)
        if is_dma:
            op.signal = True
            self.pending_dma.append(op)
        else:
            self.last_c[eng] = op
        return op

    def op(self, eng, emit, reads=(), writes=()):
        return self._add(eng, emit, list(reads), list(writes), False)

    def dma(self, q, emit, reads=(), writes=()):
        return self._add(q, emit, list(reads), list(writes), True)

    def barrier(self):
        if self.stopped:
            return
        lasts = dict(self.last_c)
        pend = list(self.pending_dma)
        self.pending_dma = []
        for eng in ALL_ENG:
            op = Op(eng, lambda e: e.nop(), False)
            for e2, o in lasts.items():
                if e2 != eng:
                    op.deps.append(o)
                    o.signal = True
            op.deps.extend(pend)
            self.streams[eng].append(op)
            if eng in COMPUTE:
                self.last_c[eng] = op

    def emit_all(self):
        nc = self.nc
        with contextlib.ExitStack() as es:
            csem = {e: es.enter_context(nc.semaphore("s_" + e)) for e in COMPUTE}
            dsem = {q: [es.enter_context(nc.semaphore(f"d_{q}_{i}")) for i in range(n)]
                    for q, n in self.n_dma_sems.items()}
            for e in COMPUTE:
                cnt = 0
                for o in self.streams[e]:
                    if o.is_dma:
                        continue
                    if o.signal:
                        cnt += 1
                        o.sem = csem[e]
                        o.val = cnt
                self.sigcount = getattr(self, "sigcount", {})
                self.sigcount[e] = (cnt, len(self.streams[e]))
            for q in self.n_dma_sems:
                k = 0
                cnts = [0] * len(dsem[q])
                for o in self.streams[q]:
                    if not o.is_dma:
                        continue
                    s = k % len(dsem[q])
                    k += 1
                    o.prewait = (dsem[q][s], cnts[s])
                    cnts[s] += 16
                    o.sem = dsem[q][s]
                    o.val = cnts[s]
            all_dma = [o for q in self.n_dma_sems for o in self.streams[q] if o.is_dma]
            block = es.enter_context(nc.Block())

            def run_stream(eng_name, eobj):
                seen = {}
                for o in self.streams[eng_name]:
                    waits = {}
                    if o.prewait is not None and o.prewait[1] > 0:
                        waits[id(o.prewait[0])] = o.prewait
                    for d in o.deps:
                        key = id(d.sem)
                        if key not in waits or waits[key][1] < d.val:
                            waits[key] = (d.sem, d.val)
                    for key, (s, v) in waits.items():
                        if seen.get(key, 0) >= v:
                            continue
                        eobj.wait_ge(s, v)
                        seen[key] = v
                    ins = o.emit(eobj)
                    if o.signal:
                        ins.then_inc(o.sem, 16 if o.is_dma else 1)
                if eng_name == SP:
                    last = {}
                    for o in all_dma:
                        key = id(o.sem)
                        if key not in last or last[key][1] < o.val:
                            last[key] = (o.sem, o.val)
                    for key, (s, v) in last.items():
                        if seen.get(key, 0) < v:
                            eobj.wait_ge(s, v)

            @block.tensor
            def _(e):
                run_stream(PE, e)

            @block.scalar
            def _(e):
                run_stream(ACT, e)

            @block.vector
            def _(e):
                run_stream(DVE, e)

            @block.gpsimd
            def _(e):
                run_stream(POOL, e)

            @block.sync
            def _(e):
                run_stream(SP, e)


FULL_CFG = dict(D=2048, TS=4096, TP=256, PAST=256, DFF=5632, DEXP=7168, GW=64)
NPR = 2
NE = 8
L = 2
MIX = 2048
INC = 4160
EPS = 1e-6
SCALE = (128 + 64) ** -0.5
C_A, C_G, C_RQ, C_RK, C_RV, C_RG, C_CQ, C_CKV, C_KR = 0, 512, 1024, 1536, 2048, 2560, 3072, 3840, 4096


class _Stop(Exception):
    pass


def build(cfg):
    def ckpt(name):
        if cfg.get("stop") == name:
            S.stopped = True
    D, TS, TP, PAST, DFF, DEXP = cfg["D"], cfg["TS"], cfg["TP"], cfg["PAST"], cfg["DFF"], cfg["DEXP"]
    KC = D // 128
    WB = min(512, D)
    nc = bass.Bass("TRN2", target_bir_lowering=False)
    S = Sched(nc)

    def inp(name, shape, dt=F32):
        return nc.dram_tensor(name, list(shape), dt, kind="ExternalInput").ap()

    def outp(name, shape, dt=F32):
        return nc.dram_tensor(name, list(shape), dt, kind="ExternalOutput").ap()

    def scr(name, shape, dt):
        kind = "ExternalOutput" if name in cfg.get("debug_out", ()) else "Internal"
        return nc.dram_tensor(name, list(shape), dt, kind=kind).ap()

    x_s = inp("x_s", [TS, D])
    x_p = inp("x_p", [NPR, TP, D])
    ckv_c = inp("ckv_c", [L, PAST, 256])
    kpe_c = inp("kpe_c", [L, PAST, 64])
    sret = inp("sret", [L, 2, 4, 128, 128])
    condT = inp("condT", [128, KC * 2])
    w_mod = inp("w_mod", [L, D, 6 * D])
    bmodT = inp("bmodT", [L, 128, 6 * KC])
    b_mod = inp("b_mod", [L, 6 * D])
    normT = inp("normT", [L, 128, 4 * KC])
    norm_g = inp("norm_g", [L, 4, D])
    w_in = inp("w_in", [L, D, INC])
    w_out = inp("w_out", [L, MIX, D])
    convwT = inp("convwT", [L, 128, 4 * 31])
    convpT = inp("convpT", [L, 128, 12])
    ret_dec = inp("ret_dec", [L, 8])
    ret_gn = inp("ret_gn", [L, 512])
    qnormT = inp("qnormT", [L, 128, 6])
    w_uq = inp("w_uq", [L, 768, 1536])
    kvnormT = inp("kvnormT", [L, 128, 2])
    kv_norm = inp("kv_norm", [L, 256])
    w_ukv = inp("w_ukv", [L, 256, 2048])
    ffn_g = inp("ffn_g", [D, DFF])
    ffn_u = inp("ffn_u", [D, DFF])
    ffn_d = inp("ffn_d", [DFF, D])
    w_rt = inp("w_rt", [D, NE])
    b_rt = inp("b_rt", [NE])
    moe_g = inp("moe_g", [NE, D, DEXP])
    moe_u = inp("moe_u", [NE, D, DEXP])
    moe_d = inp("moe_d", [NE, DEXP, D])
    c_ident = inp("c_ident", [128, 128])
    c_cos = inp("c_cos", [64, TS])
    c_sin = inp("c_sin", [64, TS])
    c_ret = inp("c_ret", [128, 6 * 128 + 2])
    y_s = outp("y_s", [TS, D])
    y_p = outp("y_p", [NPR, TP, D])
    o_ckv = outp("o_ckv", [NPR, L, TP, 256])
    o_kpe = outp("o_kpe", [NPR, L, TP, 64])
    o_sret = outp("o_sret", [NPR, L, 2, 4, 128, 128])

    seqs = [dict(T=TS, ctx=True, r=0, xin=x_s, yout=y_s)]
    for i in range(NPR):
        seqs.append(dict(T=TP, ctx=False, r=1, xin=x_p[i], yout=y_p[i], pi=i))
    for si, sq in enumerate(seqs):
        T = sq["T"]
        sq["Tk"] = T + (PAST if sq["ctx"] else 0)
        for nm, shp, dt in (("xr", [T, D], F32), ("uT", [512, T + 30], F32), ("qT", [512, T], BF16),
                            ("kT", [512, T], BF16), ("ktok", [T, 512], BF16), ("vtok", [T, 512], BF16),
                            ("sg", [T, 512], F32), ("qnT", [1024, T], BF16), ("qrT", [512, T], BF16),
                            ("nq", [8, T], F32), ("ckvnT", [256, T], BF16), ("krT", [64, T], BF16),
                            ("ymixT", [MIX, T], BF16)):
            sq[nm] = scr(f"{nm}{si}", shp, dt)
            sq["r_" + nm] = NoTrack(f"{nm}{si}")
    GD = scr("GD", [L, 2, 2, D], F32)
    r_GD = NoTrack("GD")

    groups = []
    nts = TS // 128
    for g0 in range(0, nts, 8):
        groups.append([(0, t) for t in range(g0, min(g0 + 8, nts))])
    groups.append([(1 + i, t) for i in range(NPR) for t in range(TP // 128)])

    def runs(tiles):
        out = []
        for i, (s, t) in enumerate(tiles):
            if out and out[-1][0] == s and out[-1][1] + out[-1][2] == t:
                out[-1][2] += 1
            else:
                out.append([s, t, 1, i * 128])
        return out

    with contextlib.ExitStack() as ges:
        def gsb(name, shape, dt):
            return ges.enter_context(nc.sbuf_tensor(name, list(shape), dt))

        PS = [ges.enter_context(nc.psum_tensor(f"ps{i}", [128, 512], F32)) for i in range(8)]
        RP = [Res(f"ps{i}") for i in range(8)]
        ident = gsb("ident", [128, 128], F32)
        ones_b = gsb("ones_b", [128, 128], BF16)
        ones_f = gsb("ones_f", [128, 128], F32)
        cst = gsb("cst", [128, 4], F32)
        cret = gsb("cret", [128, 6 * 128 + 2], F32)
        scT = gsb("scT", [128, KC, 2], BF16)
        modT = gsb("modT", [128, 4, KC, 2], F32)
        AB = gsb("AB", [128, 4, KC, 2], F32)
        r_const, r_scT, r_modT, r_AB = Res("const"), Res("scT"), Res("modT"), Res("AB")

        S.dma(SP, lambda e: e.dma_start(out=ident[:], in_=c_ident), [], [r_const])
        S.dma(SP, lambda e: e.dma_start(out=cret[:], in_=c_ret), [], [r_const])
        S.op(POOL, lambda e: e.memset(ones_b[:], 1.0), [], [r_const])
        S.op(POOL, lambda e: e.memset(ones_f[:], 1.0), [], [r_const])
        S.op(POOL, lambda e: e.memset(cst[:, 0:1], EPS), [], [r_const])
        S.op(POOL, lambda e: e.memset(cst[:, 1:2], 1.0), [], [r_const])
        S.op(POOL, lambda e: e.memset(cst[:, 2:3], 0.0), [], [r_const])
        eps_ap = cst[:, 0:1]

        def mm(out, lhsT, rhs, start, stop, reads, writes):
            S.op(PE, lambda e: e.matmul(out, lhsT=lhsT, rhs=rhs, start=start, stop=stop), reads, writes)

        def act(out, in_, func, reads, writes, **kw):
            S.op(ACT, lambda e: e.activation(out=out, in_=in_, func=func, **kw), reads, writes)

        def tt(eng, out, in0, in1, op, reads, writes):
            S.op(eng, lambda e: e.tensor_tensor(out=out, in0=in0, in1=in1, op=op), reads, writes)

        def ts(eng, out, in0, s1, s2, op0, op1, reads, writes, **kw):
            if s2 is None:
                S.op(eng, lambda e: e.tensor_scalar(out=out, in0=in0, scalar1=s1, scalar2=None, op0=op0, **kw), reads, writes)
            else:
                S.op(eng, lambda e: e.tensor_scalar(out=out, in0=in0, scalar1=s1, scalar2=s2, op0=op0, op1=op1, **kw),
                     reads, writes)

        def stt(out, in0, scalar, in1, op0, op1, reads, writes):
            S.op(DVE, lambda e: e.scalar_tensor_tensor(out=out, in0=in0, scalar=scalar, in1=in1, op0=op0, op1=op1),
                 reads, writes)

        def cp(eng, out, in_, reads, writes):
            if eng == ACT:
                act(out, in_, AF.Copy, reads, writes)
            else:
                S.op(eng, lambda e: e.tensor_copy(out=out, in_=in_), reads, writes)

        def dma(q, out, in_, reads, writes, **kw):
            S.dma(q, lambda e: e.dma_start(out=out, in_=in_, **kw), reads, writes)

        def transpose(out, in_, reads, writes):
            S.op(PE, lambda e: e.transpose(out=out, in_=in_, identity=ident[:]), list(reads) + [r_const], writes)

        def memset(ap, val, reads, writes):
            S.op(POOL, lambda e: e.memset(ap, val), reads, writes)

        def rmax(out, in_, reads, writes):
            S.op(DVE, lambda e: e.reduce_max(out=out, in_=in_, axis=AX.X), reads, writes)

        def recip(out, in_, reads, writes):
            S.op(DVE, lambda e: e.reciprocal(out=out, in_=in_), reads, writes)

        def bn_mean_var(out2, in_, scratch6, reads, r_scr, writes):
            S.op(DVE, lambda e: e.bn_stats(out=scratch6, in_=in_), reads, [r_scr])
            S.op(DVE, lambda e: e.bn_aggr(out=out2, in_=scratch6), [r_scr], writes)

        def rstd_from_sum(out, in_, n, reads, writes):
            act(out, in_, AF.Ln, reads + [r_const], writes, bias=eps_ap[0:out.shape[0], :], scale=1.0 / n)
            act(out, out, AF.Exp, writes, writes, scale=-0.5)

        with contextlib.ExitStack() as es:
            z = es.enter_context(nc.sbuf_tensor("zt", [128, 16], F32))
            rz = Res("zt")
            S.op(POOL, lambda e: e.memset(z[:], 0.0), [], [rz])
            for sq in seqs:
                T = sq["T"]
                for c in range(4):
                    dma(SP, sq["uT"][c * 128:(c + 1) * 128, 0:15], z[:, 0:15], [rz], [sq["r_uT"]])
                    dma(SP, sq["uT"][c * 128:(c + 1) * 128, T + 15:T + 30], z[:, 0:15], [rz], [sq["r_uT"]])
            S.barrier()

        for l in range(L if cfg.get("stop") != "init" else 0):
          try:
            last = (l == L - 1)
            with contextlib.ExitStack() as es:
                def sb(name, shape, dt):
                    return es.enter_context(nc.sbuf_tensor(f"{name}_{l}", list(shape), dt))
                ct = sb("p0ct", [128, KC * 2], F32)
                bmT = sb("p0bm", [128, 6 * KC], F32)
                nmT = sb("p0nm", [128, 4 * KC], F32)
                wts = [sb(f"p0w{i}", [128, KC, WB], BF16) for i in range(2)]
                r_w = [Res(), Res()]
                brow = sb("p0br", [2, WB], F32)
                nrow = sb("p0nr", [2, WB], F32)
                grow = sb("p0gr", [2, WB], F32)
                r_ct, r_bm, r_nm, r_br, r_nr, r_gr = [Res() for _ in range(6)]
                dma(SP, ct[:], condT, [], [r_ct])
                dma(SP, bmT[:], bmodT[l], [], [r_bm])
                dma(SP, nmT[:], normT[l], [], [r_nm])
                act(scT[:].rearrange("p k r -> p (k r)"), ct[:], AF.Silu, [r_ct], [r_scT])
                nblk = D // WB
                cpb = WB // 128
                wi = 0
                pi = 0
                for j in range(6):
                    for cb in range(nblk):
                        wt, rw = wts[wi % 2], r_w[wi % 2]
                        wi += 1
                        c0 = j * D + cb * WB
                        dma(POOL, wt[:], w_mod[l][:, c0:c0 + WB].rearrange("(k p) n -> p k n", p=128), [], [rw])
                        pb = pi % 2
                        pi += 1
                        if j in (2, 5):
                            which = 0 if j == 2 else 1
                            for k in range(KC):
                                mm(PS[pb][0:2, 0:WB], scT[:, k, :], wt[:, k, :], k == 0, k == KC - 1,
                                   [r_scT, rw], [RP[pb]])
                            dma(SP, brow[:], b_mod[l][c0:c0 + WB].partition_broadcast(2), [], [r_br])
                            dma(SP, nrow[:], norm_g[l][1 if j == 2 else 3][cb * WB:(cb + 1) * WB].partition_broadcast(2),
                                [], [r_nr])
                            tt(DVE, grow[:], PS[pb][0:2, 0:WB], brow[:], ALU.add, [RP[pb], r_br], [r_gr])
                            tt(DVE, grow[:], grow[:], nrow[:], ALU.mult, [r_gr, r_nr], [r_gr])
                            dma(SP, GD[l][which][:, cb * WB:(cb + 1) * WB], grow[:], [r_gr], [r_GD])
                        else:
                            jj = {0: 0, 1: 1, 3: 2, 4: 3}[j]
                            for cc in range(cpb):
                                for k in range(KC):
                                    mm(PS[pb][:, cc * 2:cc * 2 + 2], wt[:, k, cc * 128:(cc + 1) * 128], scT[:, k, :],
                                       k == 0, k == KC - 1, [r_scT, rw], [RP[pb]])
                            for cc in range(cpb):
                                ch = cb * cpb + cc
                                ts(DVE, modT[:, jj, ch, :], PS[pb][:, cc * 2:cc * 2 + 2], bmT[:, j * KC + ch:j * KC + ch + 1],
                                   None, ALU.add, None, [RP[pb], r_bm], [r_modT])
                for half, (jsh, jsc, ni) in enumerate(((0, 1, 0), (2, 3, 2))):
                    for r in range(2):
                        ts(DVE, AB[:, 2 * half, :, r], modT[:, jsc, :, r], 1.0, None, ALU.add, None, [r_modT], [r_AB])
                        tt(DVE, AB[:, 2 * half, :, r], AB[:, 2 * half, :, r], nmT[:, ni * KC:(ni + 1) * KC], ALU.mult,
                           [r_AB, r_nm], [r_AB])
                        cp(DVE, AB[:, 2 * half + 1, :, r], modT[:, jsh, :, r], [r_modT], [r_AB])
                S.barrier()
                ckpt(f"p0_{l}")

            def norm_transpose(xt, r_xt, xn, r_xn, ssc, r_ss, sq_scr, r_sq, hT, r_hT, col, which, r, pbanks,
                               hTf=None, r_hTf=None):
                act(sq_scr, xt, AF.Square, [r_xt], [r_sq, r_ss], accum_out=ssc)
                rstd_from_sum(ssc, ssc, D, [r_ss], [r_ss])
                ts(DVE, xn, xt, ssc, None, ALU.mult, None, [r_xt, r_ss], [r_xn])
                for k0 in range(0, KC, 4):
                    pb = pbanks[(k0 // 4) % len(pbanks)]
                    kn = min(4, KC - k0)
                    for k in range(k0, k0 + kn):
                        transpose(PS[pb][:, (k - k0) * 128:(k - k0 + 1) * 128], xn[:, k * 128:(k + 1) * 128], [r_xn], [RP[pb]])
                    for k in range(k0, k0 + kn):
                        src = PS[pb][:, (k - k0) * 128:(k - k0 + 1) * 128]
                        if (k % 2) == 0:
                            act(hT[:, k, col:col + 128], src, AF.Identity, [RP[pb], r_AB], [r_hT],
                                scale=AB[:, 2 * which, k, r:r + 1], bias=AB[:, 2 * which + 1, k, r:r + 1])
                        else:
                            ts(DVE, hT[:, k, col:col + 128], src, AB[:, 2 * which, k, r:r + 1],
                               AB[:, 2 * which + 1, k, r:r + 1], ALU.mult, ALU.add, [RP[pb], r_AB], [r_hT])
                        if hTf is not None:
                            ts(DVE, hTf[:, k, :], src, AB[:, 2 * which, k, r:r + 1],
                               AB[:, 2 * which + 1, k, r:r + 1], ALU.mult, ALU.add, [RP[pb], r_AB], [r_hTf])

            with contextlib.ExitStack() as es:
                def sb(name, shape, dt):
                    return es.enter_context(nc.sbuf_tensor(f"{name}_{l}", list(shape), dt))
                hT = sb("hT", [128, KC, 1024], BF16)
                r_hT = Res()
                xts = [sb(f"xt{i}", [128, D], F32) for i in range(2)]
                r_xts = [Res(), Res()]
                xn = sb("xn", [128, D], F32)
                r_xn = Res()
                sqs = sb("sqs", [128, D], F32)
                r_sq = Res()
                ssc = sb("ssc", [128, 16], F32)
                r_ss = Res()
                wbs = [sb(f"wb{i}", [128, KC, 512], BF16) for i in range(2)]
                r_wb = [Res(), Res()]
                wuq = sb("wuq", [128, 6, 1536], BF16)
                wuqR = sb("wuqR", [128, 6, 8, 64], BF16)
                wkr = sb("wkr", [128, KC, 64], BF16)
                wkrR = sb("wkrR", [128, KC, 64], BF16)
                r_wuq, r_wuqR, r_wkr, r_wkrR = Res(), Res(), Res(), Res()
                qnw = sb("qnw", [128, 6], F32)
                kvnw = sb("kvnw", [128, 2], F32)
                kvrow = sb("kvrow", [128, 256], F32)
                r_nw = Res()
                stf = [sb(f"stf{i}", [128, 512], F32) for i in range(4)]
                r_stf = [Res() for _ in range(4)]
                stb = [sb(f"stb{i}", [128, 512], BF16) for i in range(4)]
                r_stb = [Res() for _ in range(4)]
                cqf = sb("cqf", [128, 6, 512], F32)
                cqs = sb("cqs", [128, 6, 512], BF16)
                cqn = sb("cqn", [128, 6, 512], BF16)
                r_cqf, r_cqs, r_cqn = Res(), Res(), Res()
                rsb = sb("rsb", [128, 512], F32)
                r_rsb = Res()
                cosb = sb("cosb", [64, 512], F32)
                sinb = sb("sinb", [64, 512], F32)
                r_cs = Res()
                nqs = sb("nqs", [128, 8, 512], F32)
                r_nqs = Res()
                qsq = sb("qsq", [128, 512], BF16)
                r_qsq = Res()
                qsq2 = sb("qsq2", [128, 512], BF16)
                r_qsq2 = Res()
                memset(qsq2[:], 0.0, [], [r_qsq2])

                dma(POOL, wuq[:], w_uq[l].rearrange("(k p) n -> p k n", p=128), [], [r_wuq])
                dma(POOL, wkr[:], w_in[l][:, C_KR:C_KR + 64].rearrange("(k p) n -> p k n", p=128), [], [r_wkr])
                dma(SP, qnw[:], qnormT[l], [], [r_nw])
                dma(SP, kvnw[:], kvnormT[l], [], [r_nw])
                dma(SP, kvrow[:], kv_norm[l].partition_broadcast(128), [], [r_nw])
                wq4 = wuq[:].rearrange("p k (h c) -> p k h c", h=8)
                for (d0, s0, sgn) in ((0, 16, -1.0), (16, 0, 1.0), (32, 48, -1.0), (48, 32, 1.0)):
                    for k6 in range(6):
                        ts(DVE, wuqR[:, k6, :, d0:d0 + 16], wq4[:, k6, :, 128 + s0:128 + s0 + 16], sgn, None, ALU.mult, None,
                           [r_wuq], [r_wuqR])
                    ts(DVE, wkrR[:, :, d0:d0 + 16], wkr[:, :, s0:s0 + 16], sgn, None, ALU.mult, None, [r_wkr], [r_wkrR])

                bank_rr = [0]

                def nb():
                    b = 2 + (bank_rr[0] % 5)
                    bank_rr[0] += 1
                    return b
                st_rr = [0]

                def nst():
                    i = st_rr[0] % 4
                    st_rr[0] += 1
                    return i

                for g, tiles in enumerate(groups):
                    r = seqs[tiles[0][0]]["r"]
                    ntile = len(tiles)
                    for ti, (s, t) in enumerate(tiles):
                        sq = seqs[s]
                        xt, r_xt = xts[ti % 2], r_xts[ti % 2]
                        src = sq["xin"] if l == 0 else sq["xr"]
                        dma(SP, xt[:], src[t * 128:(t + 1) * 128, :], [sq["r_xr"]] if l else [], [r_xt])
                        norm_transpose(xt[:], r_xt, xn[:], r_xn, ssc[:, 0:1], r_ss, sqs[:], r_sq, hT, r_hT, ti * 128, 0, r,
                                       [0, 1])
                    halves = [tiles[i:i + 4] for i in range(0, ntile, 4)]
                    ckpt(f"p1nt_{l}")
                    for blk in range(8):
                        ckpt(f"p1b{blk}_{l}")
                        wb, rwb = wbs[blk % 2], r_wb[blk % 2]
                        c0 = blk * 512
                        dma(POOL, wb[:], w_in[l][:, c0:c0 + 512].rearrange("(k p) n -> p k n", p=128), [], [rwb])
                        for hi, half in enumerate(halves):
                            n = len(half) * 128
                            hc0 = hi * 512
                            hruns = runs(half)

                            def fm(cc, pb, wsrc=None, rws=None, M=128, wcols=None):
                                for k in range(KC):
                                    lhsT = wb[:, k, cc * 128:cc * 128 + M] if wsrc is None else wsrc[:, k, wcols[0]:wcols[1]]
                                    mm(PS[pb][0:M, 0:n], lhsT, hT[:, k, hc0:hc0 + n], k == 0, k == KC - 1,
                                       [r_hT, rwb if rws is None else rws], [RP[pb]])

                            def store_fm(buf, rbuf, M, dst_name, row0):
                                for (s, t0, nt, col) in hruns:
                                    sq = seqs[s]
                                    dst = sq[dst_name][row0:row0 + M, t0 * 128:(t0 + nt) * 128] if dst_name != "uT" else \
                                        sq[dst_name][row0:row0 + M, 15 + t0 * 128:15 + (t0 + nt) * 128]
                                    dma(SP, dst, buf[0:M, col:col + nt * 128], [rbuf], [sq["r_" + dst_name]])

                            if blk == 0:
                                pass
                            if blk in (0, 6):
                                continue
                            if blk == 1:
                                wa, rwa = wbs[0], r_wb[0]
                                for cc in range(4):
                                    pa, pg = nb(), nb()
                                    fm(cc, pa, wsrc=wa, rws=rwa, wcols=(cc * 128, cc * 128 + 128))
                                    fm(cc, pg)
                                    i = nst()
                                    act(stf[i][:, 0:n], PS[pg][:, 0:n], AF.Sigmoid, [RP[pg]], [r_stf[i]])
                                    tt(DVE, stf[i][:, 0:n], PS[pa][:, 0:n], stf[i][:, 0:n], ALU.mult, [RP[pa], r_stf[i]],
                                       [r_stf[i]])
                                    store_fm(stf[i], r_stf[i], 128, "uT", cc * 128)
                            elif blk == 2:
                                for cc in range(4):
                                    pb = nb()
                                    fm(cc, pb)
                                    i = nst()
                                    cp(ACT if cc % 2 else DVE, stb[i][:, 0:n], PS[pb][:, 0:n], [RP[pb]], [r_stb[i]])
                                    store_fm(stb[i], r_stb[i], 128, "qT", cc * 128)
                            elif blk == 3:
                                for cc in range(4):
                                    pb = nb()
                                    fm(cc, pb)
                                    i = nst()
                                    act(stb[i][:, 0:n], PS[pb][:, 0:n], AF.Copy, [RP[pb]], [r_stb[i]], scale=128 ** -0.5)
                                    store_fm(stb[i], r_stb[i], 128, "kT", cc * 128)
                                for ti2, (s, t) in enumerate(half):
                                    pb = nb()
                                    for k in range(KC):
                                        mm(PS[pb][:, 0:512], hT[:, k, hc0 + ti2 * 128:hc0 + (ti2 + 1) * 128], wb[:, k, :],
                                           k == 0, k == KC - 1, [r_hT, rwb], [RP[pb]])
                                    i = nst()
                                    act(stb[i][:, :], PS[pb][:, :], AF.Copy, [RP[pb]], [r_stb[i]], scale=128 ** -0.5)
                                    dma(SP, seqs[s]["ktok"][t * 128:(t + 1) * 128, :], stb[i][:, :], [r_stb[i]],
                                        [seqs[s]["r_ktok"]])
                            elif blk in (4, 5):
                                for ti2, (s, t) in enumerate(half):
                                    pb = nb()
                                    for k in range(KC):
                                        mm(PS[pb][:, 0:512], hT[:, k, hc0 + ti2 * 128:hc0 + (ti2 + 1) * 128], wb[:, k, :],
                                           k == 0, k == KC - 1, [r_hT, rwb], [RP[pb]])
                                    i = nst()
                                    if blk == 4:
                                        cp(DVE, stb[i][:, :], PS[pb][:, :], [RP[pb]], [r_stb[i]])
                                        dma(SP, seqs[s]["vtok"][t * 128:(t + 1) * 128, :], stb[i][:, :], [r_stb[i]],
                                            [seqs[s]["r_vtok"]])
                                    else:
                                        act(stf[i][:, :], PS[pb][:, :], AF.Silu, [RP[pb]], [r_stf[i]])
                                        dma(SP, seqs[s]["sg"][t * 128:(t + 1) * 128, :], stf[i][:, :], [r_stf[i]],
                                            [seqs[s]["r_sg"]])
                            elif blk == 6:
                                for cc in range(4):
                                    pb = nb()
                                    fm(cc, pb)
                                    cp(ACT if cc % 2 else DVE, cqf[:, cc, 0:n], PS[pb][:, 0:n], [RP[pb]], [r_cqf])
                            elif blk == 7:
                                w6, rw6 = wbs[0], r_wb[0]
                                for cc in range(4):
                                    pb = nb()
                                    fm(cc, pb, wsrc=w6, rws=rw6, wcols=(cc * 128, cc * 128 + 128))
                                    cp(ACT if cc % 2 else DVE, cqf[:, cc, 0:n], PS[pb][:, 0:n], [RP[pb]], [r_cqf])
                                for cc in range(2):
                                    pb = nb()
                                    fm(cc, pb)
                                    cp(ACT if cc % 2 else DVE, cqf[:, 4 + cc, 0:n], PS[pb][:, 0:n], [RP[pb]], [r_cqf])
                                act(cqs[:, :, 0:n], cqf[:, :, 0:n], AF.Square, [r_cqf], [r_cqs])
                                pz = nb()
                                for cc in range(6):
                                    mm(PS[pz][:, 0:n], ones_b[:], cqs[:, cc, 0:n], cc == 0, cc == 5, [r_cqs, r_const], [RP[pz]])
                                rstd_from_sum(rsb[:, 0:n], PS[pz][:, 0:n], 768, [RP[pz]], [r_rsb])
                                for cc in range(6):
                                    stt(cqn[:, cc, 0:n], cqf[:, cc, 0:n], qnw[:, cc:cc + 1], rsb[:, 0:n], ALU.mult, ALU.mult,
                                        [r_cqf, r_nw, r_rsb], [r_cqn])
                                ckpt(f"p1x1_{l}")
                                for cc in range(2):
                                    pb = nb()
                                    fm(2 + cc, pb)
                                    cp(ACT if cc % 2 else DVE, cqf[:, cc, 0:n], PS[pb][:, 0:n], [RP[pb]], [r_cqf])
                                act(cqs[:, 0:2, 0:n], cqf[:, 0:2, 0:n], AF.Square, [r_cqf], [r_cqs])
                                pz = nb()
                                for cc in range(2):
                                    mm(PS[pz][:, 0:n], ones_b[:], cqs[:, cc, 0:n], cc == 0, cc == 1, [r_cqs, r_const], [RP[pz]])
                                rstd_from_sum(rsb[:, 0:n], PS[pz][:, 0:n], 256, [RP[pz]], [r_rsb])
                                for cc in range(2):
                                    i = nst()
                                    stt(stb[i][:, 0:n], cqf[:, cc, 0:n], kvnw[:, cc:cc + 1], rsb[:, 0:n], ALU.mult, ALU.mult,
                                        [r_cqf, r_nw, r_rsb], [r_stb[i]])
                                    store_fm(stb[i], r_stb[i], 128, "ckvnT", cc * 128)
                                ckpt(f"p1x2_{l}")
                                is_ctx = seqs[half[0][0]]["ctx"]
                                if is_ctx:
                                    t0 = half[0][1] * 128
                                    dma(SP, cosb[:, 0:n], c_cos[:, t0:t0 + n], [], [r_cs])
                                    dma(SP, sinb[:, 0:n], c_sin[:, t0:t0 + n], [], [r_cs])

                                def rope_evac(pa, pr, dst, rdst, M=64):
                                    i1, i2 = nst(), nst()
                                    tt(DVE, stf[i1][0:M, 0:n], PS[pa][0:M, 0:n], cosb[0:M, 0:n], ALU.mult, [RP[pa], r_cs],
                                       [r_stf[i1]])
                                    tt(DVE, stf[i2][0:M, 0:n], PS[pr][0:M, 0:n], sinb[0:M, 0:n], ALU.mult, [RP[pr], r_cs],
                                       [r_stf[i2]])
                                    tt(POOL, dst, stf[i1][0:M, 0:n], stf[i2][0:M, 0:n], ALU.add, [r_stf[i1], r_stf[i2]],
                                       [rdst])

                                pa = nb()
                                fm(0, pa, wsrc=wkr, rws=r_wkr, M=64, wcols=(0, 64))
                                i = nst()
                                if is_ctx:
                                    pr = nb()
                                    fm(0, pr, wsrc=wkrR, rws=r_wkrR, M=64, wcols=(0, 64))
                                    rope_evac(pa, pr, stb[i][0:64, 0:n], r_stb[i])
                                else:
                                    cp(DVE, stb[i][0:64, 0:n], PS[pa][0:64, 0:n], [RP[pa]], [r_stb[i]])
                                store_fm(stb[i], r_stb[i], 64, "krT", 0)
                                ckpt(f"p1x3_{l}")
                                for h in range(8):
                                    pn = nb()
                                    for cc in range(6):
                                        mm(PS[pn][:, 0:n], wuq[:, cc, h * 192:h * 192 + 128], cqn[:, cc, 0:n], cc == 0, cc == 5,
                                           [r_wuq, r_cqn], [RP[pn]])
                                    i = nst()
                                    cp(ACT, stb[i][:, 0:n], PS[pn][:, 0:n], [RP[pn]], [r_stb[i]])
                                    store_fm(stb[i], r_stb[i], 128, "qnT", h * 128)
                                    ckpt(f"p1y1_{h}_{l}")
                                    act(qsq[:, 0:n], PS[pn][:, 0:n], AF.Square, [RP[pn]], [r_qsq])
                                    pq = nb()
                                    ckpt(f"p1y2_{h}_{l}")
                                    pa = nb()
                                    for cc in range(6):
                                        mm(PS[pa][0:64, 0:n], wuq[:, cc, h * 192 + 128:h * 192 + 192], cqn[:, cc, 0:n], cc == 0,
                                           cc == 5, [r_wuq, r_cqn], [RP[pa]])
                                    i = nst()
                                    if is_ctx:
                                        pr = nb()
                                        for cc in range(6):
                                            mm(PS[pr][0:64, 0:n], wuqR[:, cc, h, :], cqn[:, cc, 0:n], cc == 0, cc == 5,
                                               [r_wuqR, r_cqn], [RP[pr]])
                                        rope_evac(pa, pr, stb[i][0:64, 0:n], r_stb[i])
                                    else:
                                        cp(DVE, stb[i][0:64, 0:n], PS[pa][0:64, 0:n], [RP[pa]], [r_stb[i]])
                                    store_fm(stb[i], r_stb[i], 64, "qrT", h * 64)
                                    ckpt(f"p1y3_{h}_{l}")
                                    tt(POOL, qsq2[0:64, 0:n], stb[i][0:64, 0:n], stb[i][0:64, 0:n], ALU.mult, [r_stb[i]], [r_qsq2])
                                    ckpt(f"p1y3a_{h}_{l}")
                                    mm(PS[pq][:, 0:n], ones_b[:], qsq[:, 0:n], True, False, [r_qsq, r_const], [RP[pq]])
                                    ckpt(f"p1y3b_{h}_{l}")
                                    mm(PS[pq][:, 0:n], ones_b[:], qsq2[:, 0:n], False, True, [r_qsq2, r_const], [RP[pq]])
                                    ckpt(f"p1y4_{h}_{l}")
                                    act(nqs[:, h, 0:n], PS[pq][:, 0:n], AF.Ln, [RP[pq]], [r_nqs])
                                    act(nqs[:, h, 0:n], nqs[:, h, 0:n], AF.Exp, [r_nqs], [r_nqs], scale=0.5)
                                    ckpt(f"p1y5_{h}_{l}")
                                ckpt(f"p1x4_{l}")
                                for (s, t0, nt, col) in hruns:
                                    sq = seqs[s]
                                    dma(SP, sq["nq"][:, t0 * 128:(t0 + nt) * 128].rearrange("(o h) t -> o h t", o=1), nqs[0:1, :, col:col + nt * 128],
                                        [r_nqs], [sq["r_nq"]])
                                ckpt(f"p1x5_{l}")
                                if not is_ctx:
                                    for ti2, (s, t) in enumerate(half):
                                        sq = seqs[s]
                                        pb = nb()
                                        for k in range(KC):
                                            mm(PS[pb][:, 0:256], hT[:, k, hc0 + ti2 * 128:hc0 + (ti2 + 1) * 128],
                                               wb[:, k, 256:512], k == 0, k == KC - 1, [r_hT, rwb], [RP[pb]])
                                        for k in range(KC):
                                            mm(PS[pb][:, 256:320], hT[:, k, hc0 + ti2 * 128:hc0 + (ti2 + 1) * 128],
                                               wkr[:, k, :], k == 0, k == KC - 1, [r_hT, r_wkr], [RP[pb]])
                                        i = nst()
                                        act(stf[i][:, 0:256], PS[pb][:, 0:256], AF.Square, [RP[pb]], [r_stf[i], r_ss],
                                            accum_out=ssc[:, 1:2])
                                        rstd_from_sum(ssc[:, 1:2], ssc[:, 1:2], 256, [r_ss], [r_ss])
                                        stt(stf[i][:, 0:256], PS[pb][:, 0:256], ssc[:, 1:2], kvrow[:, :], ALU.mult, ALU.mult,
                                            [RP[pb], r_ss, r_nw], [r_stf[i]])
                                        cp(ACT, stf[i][:, 256:320], PS[pb][:, 256:320], [RP[pb]], [r_stf[i]])
                                        dma(SP, o_ckv[sq["pi"]][l][t * 128:(t + 1) * 128, :], stf[i][:, 0:256], [r_stf[i]], [Res()])
                                        dma(SP, o_kpe[sq["pi"]][l][t * 128:(t + 1) * 128, :], stf[i][:, 256:320], [r_stf[i]], [Res()])
                S.barrier()
                ckpt(f"p1_{l}")

            for si, sq in enumerate(seqs):
                T = sq["T"]
                with contextlib.ExitStack() as es:
                    def sb(name, shape, dt):
                        return es.enter_context(nc.sbuf_tensor(f"{name}_{l}_{si}", list(shape), dt))
                    uts = [sb(f"ut{i}", [128, T + 30], F32) for i in range(2)]
                    r_ut = [Res(), Res()]
                    cv = sb("cv", [128, 4, T], F32)
                    r_cv = [Res() for _ in range(4)]
                    cw = sb("cw", [128, 4 * 31], F32)
                    cpar = sb("cpar", [128, 12], F32)
                    r_cw = Res()
                    sqf = sb("sqf", [128, 512], F32)
                    r_sqf = Res()
                    mean = sb("mean", [128, 512], F32)
                    msq = sb("msq", [128, 512], F32)
                    rst = sb("rst", [128, 512], F32)
                    r_mean, r_rst = Res(), Res()
                    tmp = [sb(f"ctmp{i}", [128, 512], F32) for i in range(2)]
                    r_tmp = [Res(), Res()]
                    yo = [sb(f"cyo{i}", [128, 512], BF16) for i in range(2)]
                    r_yo = [Res(), Res()]
                    dma(SP, cw[:], convwT[l], [], [r_cw])
                    dma(SP, cpar[:], convpT[l], [], [r_cw])
                    for c in range(4):
                        ut, rut = uts[c % 2], r_ut[c % 2]
                        dma(SP, ut[:], sq["uT"][c * 128:(c + 1) * 128, :], [sq["r_uT"]], [rut])
                        ts(DVE, cv[:, c, :], ut[:, 0:T], cw[:, c * 31:c * 31 + 1], cpar[:, c:c + 1], ALU.mult, ALU.add,
                           [rut, r_cw], [r_cv[c]])
                        for k in range(1, 31):
                            stt(cv[:, c, :], ut[:, k:k + T], cw[:, c * 31 + k:c * 31 + k + 1], cv[:, c, :], ALU.mult, ALU.add,
                                [rut, r_cw, r_cv[c]], [r_cv[c]])
                    for b0 in range(0, T, 256):
                        n = min(256, T - b0)
                        p_s, p_q = 0, 1
                        for c in range(4):
                            mm(PS[p_s][:, 0:n], ones_f[:], cv[:, c, b0:b0 + n], c == 0, c == 3, [r_cv[c], r_const], [RP[p_s]])
                        for c in range(4):
                            act(sqf[:, 0:n], cv[:, c, b0:b0 + n], AF.Square, [r_cv[c]], [r_sqf])
                            mm(PS[p_q][:, 0:n], ones_f[:], sqf[:, 0:n], c == 0, c == 3, [r_sqf, r_const], [RP[p_q]])
                        ts(DVE, mean[:, 0:n], PS[p_s][:, 0:n], 1.0 / 512, None, ALU.mult, None, [RP[p_s]], [r_mean])
                        tt(DVE, msq[:, 0:n], mean[:, 0:n], mean[:, 0:n], ALU.mult, [r_mean], [r_mean])
                        stt(rst[:, 0:n], PS[p_q][:, 0:n], 1.0 / 512, msq[:, 0:n], ALU.mult, ALU.subtract, [RP[p_q], r_mean],
                            [r_rst])
                        rstd_from_sum(rst[:, 0:n], rst[:, 0:n], 1.0, [r_rst], [r_rst])
                        for c in range(4):
                            tm, rtm = tmp[c % 2], r_tmp[c % 2]
                            tt(DVE, tm[:, 0:n], cv[:, c, b0:b0 + n], mean[:, 0:n], ALU.subtract, [r_cv[c], r_mean], [rtm])
                            tt(POOL, tm[:, 0:n], tm[:, 0:n], rst[:, 0:n], ALU.mult, [rtm, r_rst], [rtm])
                            y_, ry = yo[c % 2], r_yo[c % 2]
                            act(y_[:, 0:n], tm[:, 0:n], AF.Silu, [rtm, r_cw], [ry], scale=cpar[:, 4 + c:5 + c],
                                bias=cpar[:, 8 + c:9 + c])
                            dma(SP, sq["ymixT"][c * 128:(c + 1) * 128, b0:b0 + n], y_[:, 0:n], [ry], [sq["r_ymixT"]])
                    S.barrier()
                    ckpt(f"p2a_{l}")

            with contextlib.ExitStack() as es0:
                lg = es0.enter_context(nc.sbuf_tensor(f"lg_{l}", [128, 8], F32))
                r_lg = Res()
                dma(SP, lg[:], ret_dec[l].partition_broadcast(128), [], [r_lg])
                act(lg[:], lg[:], AF.Exp, [r_lg], [r_lg], scale=-1.0)
                act(lg[:], lg[:], AF.Ln, [r_lg, r_const], [r_lg], bias=cst[:, 1:2], scale=1.0)
                ts(DVE, lg[:], lg[:], -1.0, None, ALU.mult, None, [r_lg], [r_lg])
                gnrow = es0.enter_context(nc.sbuf_tensor(f"gnrow_{l}", [128, 512], F32))
                r_gn = Res()
                dma(SP, gnrow[:], ret_gn[l].partition_broadcast(128), [], [r_gn])
                for h in range(4):
                    with contextlib.ExitStack() as es1:
                        def sb1(name, shape, dt):
                            return es1.enter_context(nc.sbuf_tensor(f"{name}_{l}_{h}", list(shape), dt))
                        maskT = sb1("maskT", [128, 128], F32)
                        mtmp = sb1("mtmp", [128, 128], F32)
                        qdF = sb1("qdF", [128, 128], F32)
                        qdB = sb1("qdB", [128, 128], F32)
                        kdc = sb1("kdc", [128, 4], F32)
                        r_dc = Res()
                        lf, lb = lg[:, h:h + 1], lg[:, 4 + h:5 + h]
                        act(maskT[:], cret[:, 0:128], AF.Exp, [r_const, r_lg], [r_dc], scale=lf)
                        tt(DVE, maskT[:], maskT[:], cret[:, 128:256], ALU.mult, [r_dc, r_const], [r_dc])
                        act(mtmp[:], cret[:, 256:384], AF.Exp, [r_const, r_lg], [r_dc], scale=lb)
                        tt(DVE, mtmp[:], mtmp[:], cret[:, 384:512], ALU.mult, [r_dc, r_const], [r_dc])
                        tt(DVE, maskT[:], maskT[:], mtmp[:], ALU.add, [r_dc], [r_dc])
                        act(qdF[:], cret[:, 512:640], AF.Exp, [r_const, r_lg], [r_dc], scale=lf)
                        act(qdB[:], cret[:, 640:768], AF.Exp, [r_const, r_lg], [r_dc], scale=lb)
                        act(kdc[:, 0:1], cret[:, 768:769], AF.Exp, [r_const, r_lg], [r_dc], scale=lf)
                        act(kdc[:, 1:2], cret[:, 769:770], AF.Exp, [r_const, r_lg], [r_dc], scale=lb)
                        act(kdc[:, 2:3], lg[:, h:h + 1], AF.Exp, [r_lg], [r_dc], scale=128.0)
                        act(kdc[:, 3:4], lg[:, 4 + h:5 + h], AF.Exp, [r_lg], [r_dc], scale=128.0)
                        for si, sq in enumerate(seqs):
                            T = sq["T"]
                            nch = T // 128
                            with contextlib.ExitStack() as es:
                                def sb(name, shape, dt):
                                    return es.enter_context(nc.sbuf_tensor(f"{name}_{l}_{h}_{si}", list(shape), dt))
                                qT = sb("rqT", [128, T], BF16)
                                kT = sb("rkT", [128, T], BF16)
                                kt = sb("rkt", [128, nch, 128], BF16)
                                vt = sb("rvt", [128, nch, 128], BF16)
                                sgt = sb("rsg", [128, nch, 128], F32)
                                r_in = Res()
                                SfI = sb("SfI", [128, nch, 128], BF16)
                                SbI = sb("SbI", [128, nch, 128], BF16)
                                r_SfI, r_SbI = Res(), Res()
                                Sf = sb("Sf", [128, 128], F32)
                                Sb = sb("Sb", [128, 128], F32)
                                r_Sf, r_Sb = Res(), Res()
                                kds = [sb(f"kds{i}", [128, 128], BF16) for i in range(4)]
                                r_kds = [Res() for _ in range(4)]
                                yT = sb("ryT", [128, T], BF16)
                                r_yT = Res()
                                dma(SP, qT[:], sq["qT"][h * 128:(h + 1) * 128, :], [sq["r_qT"]], [r_in])
                                dma(SP, kT[:], sq["kT"][h * 128:(h + 1) * 128, :], [sq["r_kT"]], [r_in])
                                dma(SP, kt[:], sq["ktok"][:, h * 128:(h + 1) * 128].rearrange("(c p) d -> p c d", p=128),
                                    [sq["r_ktok"]], [r_in])
                                dma(SP, vt[:], sq["vtok"][:, h * 128:(h + 1) * 128].rearrange("(c p) d -> p c d", p=128),
                                    [sq["r_vtok"]], [r_in])
                                dma(SP, sgt[:], sq["sg"][:, h * 128:(h + 1) * 128].rearrange("(c p) d -> p c d", p=128),
                                    [sq["r_sg"]], [r_in])
                                if sq["ctx"]:
                                    dma(SP, Sf[:], sret[l][0][h], [], [r_Sf])
                                    dma(SP, Sb[:], sret[l][1][h], [], [r_Sb])
                                else:
                                    memset(Sf[:], 0.0, [], [r_Sf])
                                    memset(Sb[:], 0.0, [], [r_Sb])
                                for i in range(nch):
                                    cf, cb_ = i, nch - 1 - i
                                    cp(ACT, SfI[:, cf, :], Sf[:], [r_Sf], [r_SfI])
                                    cp(ACT, SbI[:, cb_, :], Sb[:], [r_Sb], [r_SbI])
                                    kf, rkf = kds[(2 * i) % 4], r_kds[(2 * i) % 4]
                                    kb, rkb = kds[(2 * i + 1) % 4], r_kds[(2 * i + 1) % 4]
                                    ts(POOL, kf[:], kt[:, cf, :], kdc[:, 0:1], None, ALU.mult, None, [r_in, r_dc], [rkf])
                                    ts(POOL, kb[:], kt[:, cb_, :], kdc[:, 1:2], None, ALU.mult, None, [r_in, r_dc], [rkb])
                                    mm(PS[0][:, 0:128], kf[:], vt[:, cf, :], True, True, [rkf, r_in], [RP[0]])
                                    mm(PS[1][:, 0:128], kb[:], vt[:, cb_, :], True, True, [rkb, r_in], [RP[1]])
                                    stt(Sf[:], Sf[:], kdc[:, 2:3], PS[0][:, 0:128], ALU.mult, ALU.add, [r_Sf, r_dc, RP[0]], [r_Sf])
                                    stt(Sb[:], Sb[:], kdc[:, 3:4], PS[1][:, 0:128], ALU.mult, ALU.add, [r_Sb, r_dc, RP[1]], [r_Sb])
                                if not sq["ctx"]:
                                    dma(SP, o_sret[sq["pi"]][l][0][h], Sf[:], [r_Sf], [Res()])
                                    dma(SP, o_sret[sq["pi"]][l][1][h], Sb[:], [r_Sb], [Res()])
                                PTs = [sb(f"PT{i}", [128, 128], BF16) for i in range(2)]
                                qFs = [sb(f"qF{i}", [128, 128], BF16) for i in range(2)]
                                qBs = [sb(f"qB{i}", [128, 128], BF16) for i in range(2)]
                                ons = [sb(f"on{i}", [128, 128], F32) for i in range(2)]
                                sts = [sb(f"rst{i}", [128, 4], F32) for i in range(2)]
                                jk = [sb(f"rjk{i}", [128, 128], F32) for i in range(2)]
                                r_PT, r_qF, r_qB, r_on, r_st, r_jk = ([Res(), Res()] for _ in range(6))
                                for c in range(nch):
                                    b = c % 2
                                    pa, po, pt_ = 2 + b, 4 + b, 6 + b
                                    cs = slice(c * 128, (c + 1) * 128)
                                    mm(PS[pa][:, 0:128], kT[:, cs], qT[:, cs], True, True, [r_in], [RP[pa]])
                                    tt(DVE, PTs[b][:], PS[pa][:, 0:128], maskT[:], ALU.mult, [RP[pa], r_dc], [r_PT[b]])
                                    tt(POOL, qFs[b][:], qT[:, cs], qdF[:], ALU.mult, [r_in, r_dc], [r_qF[b]])
                                    tt(POOL, qBs[b][:], qT[:, cs], qdB[:], ALU.mult, [r_in, r_dc], [r_qB[b]])
                                    mm(PS[po][:, 0:128], PTs[b][:], vt[:, c, :], True, False, [r_PT[b], r_in], [RP[po]])
                                    mm(PS[po][:, 0:128], qFs[b][:], SfI[:, c, :], False, False, [r_qF[b], r_SfI], [RP[po]])
                                    mm(PS[po][:, 0:128], qBs[b][:], SbI[:, c, :], False, True, [r_qB[b], r_SbI], [RP[po]])
                                    bn_mean_var(sts[b][:, 0:2], PS[po][:, 0:128], jk[b][:, 0:6], [RP[po]], r_jk[b], [r_st[b]])
                                    rstd_from_sum(sts[b][:, 1:2], sts[b][:, 1:2], 1.0, [r_st[b]], [r_st[b]])
                                    ts(DVE, ons[b][:], PS[po][:, 0:128], sts[b][:, 0:1], sts[b][:, 1:2], ALU.subtract, ALU.mult,
                                       [RP[po], r_st[b]], [r_on[b]])
                                    tt(POOL, ons[b][:], ons[b][:], gnrow[:, h * 128:(h + 1) * 128], ALU.mult, [r_on[b], r_gn],
                                       [r_on[b]])
                                    tt(POOL, ons[b][:], ons[b][:], sgt[:, c, :], ALU.mult, [r_on[b], r_in], [r_on[b]])
                                    transpose(PS[pt_][:, 0:128], ons[b][:], [r_on[b]], [RP[pt_]])
                                    cp(ACT, yT[:, cs], PS[pt_][:, 0:128], [RP[pt_]], [r_yT])
                                dma(SP, sq["ymixT"][512 + h * 128:512 + (h + 1) * 128, :], yT[:], [r_yT], [sq["r_ymixT"]])
                                S.barrier()
                                ckpt(f"p2b_{l}")

            for si, sq in enumerate(seqs):
                T, Tk = sq["T"], sq["Tk"]
                nkc = Tk // 128
                off = Tk - T
                with contextlib.ExitStack() as es0:
                    def sb0(name, shape, dt):
                        return es0.enter_context(nc.sbuf_tensor(f"{name}_{l}_{si}", list(shape), dt))
                    ckA = sb0("ckA", [128, 2, Tk], BF16)
                    krA = sb0("krA", [128, Tk], BF16)
                    krsq = sb0("krsq", [128, Tk], BF16)
                    wkv = sb0("wkv", [128, 2, 2048], BF16)
                    r_ckA, r_krA, r_krsq, r_wkv = Res(), Res(), Res(), Res()
                    dma(POOL, wkv[:], w_ukv[l].rearrange("(k p) n -> p k n", p=128), [], [r_wkv])
                    dma(SP, ckA[:, :, off:Tk], sq["ckvnT"].rearrange("(k p) t -> p k t", p=128), [sq["r_ckvnT"]], [r_ckA])
                    memset(krA[:], 0.0, [], [r_krA])
                    memset(krA[64:65, :], 1.0, [r_krA], [r_krA])
                    dma(SP, krA[0:64, off:Tk], sq["krT"], [sq["r_krT"]], [r_krA])
                    if sq["ctx"]:
                        with contextlib.ExitStack() as es:
                            cin = es.enter_context(nc.sbuf_tensor(f"cin_{l}", [128, 320], F32))
                            r_cin = Res()
                            for kt_ in range(PAST // 128):
                                dma(SP, cin[:, 0:256], ckv_c[l][kt_ * 128:(kt_ + 1) * 128, :], [], [r_cin])
                                dma(SP, cin[:, 256:320], kpe_c[l][kt_ * 128:(kt_ + 1) * 128, :], [], [r_cin])
                                for cc in range(2):
                                    transpose(PS[cc][:, 0:128], cin[:, cc * 128:(cc + 1) * 128], [r_cin], [RP[cc]])
                                    cp(DVE, ckA[:, cc, kt_ * 128:(kt_ + 1) * 128], PS[cc][:, 0:128], [RP[cc]], [r_ckA])
                                transpose(PS[2][0:64, 0:128], cin[:, 256:320], [r_cin], [RP[2]])
                                cp(DVE, krA[0:64, kt_ * 128:(kt_ + 1) * 128], PS[2][0:64, 0:128], [RP[2]], [r_krA])
                            S.barrier()
                    tt(POOL, krsq[:], krA[:], krA[:], ALU.mult, [r_krA], [r_krsq])
                    for h in range(8):
                        with contextlib.ExitStack() as es:
                            def sb(name, shape, dt):
                                return es.enter_context(nc.sbuf_tensor(f"{name}_{l}_{si}_{h}", list(shape), dt))
                            KnT = sb("KnT", [128, Tk], BF16)
                            Ksq = sb("Ksq", [128, 512], BF16)
                            Vh = sb("Vh", [128, nkc, 128], BF16)
                            qn = sb("qn", [128, T], BF16)
                            qr = sb("qr", [128, T], BF16)
                            nqt = sb("nqt", [65, T], F32)
                            kmx = sb("kmx", [128, 16], F32)
                            r_KnT, r_Ksq, r_Vh, r_qn, r_qr, r_nqt, r_kmx = [Res() for _ in range(7)]
                            dma(SP, qn[:], sq["qnT"][h * 128:(h + 1) * 128, :], [sq["r_qnT"]], [r_qn])
                            memset(qr[:], 0.0, [], [r_qr])
                            dma(SP, qr[0:64, :], sq["qrT"][h * 64:(h + 1) * 64, :], [sq["r_qrT"]], [r_qr])
                            dma(SP, nqt[64:65, :], sq["nq"][h:h + 1, :], [sq["r_nq"]], [r_nqt])
                            nblk = (Tk + 511) // 512
                            for b in range(nblk):
                                k0 = b * 512
                                n = min(512, Tk - k0)
                                pb = b % 2
                                for cc in range(2):
                                    mm(PS[pb][:, 0:n], wkv[:, cc, h * 256:h * 256 + 128], ckA[:, cc, k0:k0 + n], cc == 0, cc == 1,
                                       [r_wkv, r_ckA], [RP[pb]])
                                cp(ACT, KnT[:, k0:k0 + n], PS[pb][:, 0:n], [RP[pb]], [r_KnT])
                                act(Ksq[:, 0:n], PS[pb][:, 0:n], AF.Square, [RP[pb]], [r_Ksq])
                                pz = 2 + b % 2
                                mm(PS[pz][:, 0:n], ones_b[:], Ksq[:, 0:n], True, False, [r_Ksq, r_const], [RP[pz]])
                                mm(PS[pz][:, 0:n], ones_b[:], krsq[:, k0:k0 + n], False, True, [r_krsq, r_const], [RP[pz]])
                                rmax(kmx[:, b:b + 1], PS[pz][:, 0:n], [RP[pz]], [r_kmx])
                            rmax(kmx[:, 15:16], kmx[:, 0:nblk], [r_kmx], [r_kmx])
                            act(kmx[:, 15:16], kmx[:, 15:16], AF.Ln, [r_kmx], [r_kmx])
                            act(kmx[:, 15:16], kmx[:, 15:16], AF.Exp, [r_kmx], [r_kmx], scale=0.5)
                            ts(DVE, kmx[:, 14:15], kmx[:, 15:16], -1.0, None, ALU.mult, None, [r_kmx], [r_kmx])
                            ts(DVE, qr[64:65, :], nqt[64:65, :], kmx[64:65, 14:15], None, ALU.mult, None, [r_nqt, r_kmx], [r_qr])
                            for kc in range(nkc):
                                pb = 4 + kc % 2
                                for cc in range(2):
                                    mm(PS[pb][:, 0:128], ckA[:, cc, kc * 128:(kc + 1) * 128],
                                       wkv[:, cc, h * 256 + 128:h * 256 + 256], cc == 0, cc == 1, [r_wkv, r_ckA], [RP[pb]])
                                cp(DVE if kc % 2 else ACT, Vh[:, kc, :], PS[pb][:, 0:128], [RP[pb]], [r_Vh])
                            PTb = [sb(f"PTb{i}", [128, 512], BF16) for i in range(3)]
                            r_PTb = [Res() for _ in range(3)]
                            rz = sb("rz", [128, 512], F32)
                            r_rz = Res()
                            ob = [sb(f"ob{i}", [128, 512], BF16) for i in range(2)]
                            r_ob = [Res(), Res()]
                            QG = min(512, T)
                            for qi, q0 in enumerate(range(0, T, QG)):
                                po, pzz = 6, 7
                                for kc in range(nkc):
                                    ps_ = kc % 3
                                    ks = slice(kc * 128, (kc + 1) * 128)
                                    mm(PS[ps_][:, 0:QG], KnT[:, ks], qn[:, q0:q0 + QG], True, False, [r_KnT, r_qn], [RP[ps_]])
                                    mm(PS[ps_][:, 0:QG], krA[:, ks], qr[:, q0:q0 + QG], False, True, [r_krA, r_qr], [RP[ps_]])
                                    act(PTb[ps_][:, 0:QG], PS[ps_][:, 0:QG], AF.Exp, [RP[ps_]], [r_PTb[ps_]], scale=SCALE)
                                    mm(PS[po][:, 0:QG], Vh[:, kc, :], PTb[ps_][:, 0:QG], kc == 0, kc == nkc - 1,
                                       [r_Vh, r_PTb[ps_]], [RP[po]])
                                    mm(PS[pzz][:, 0:QG], ones_b[:], PTb[ps_][:, 0:QG], kc == 0, kc == nkc - 1,
                                       [r_const, r_PTb[ps_]], [RP[pzz]])
                                recip(rz[:, 0:QG], PS[pzz][:, 0:QG], [RP[pzz]], [r_rz])
                                o_, ro = ob[qi % 2], r_ob[qi % 2]
                                tt(DVE, o_[:, 0:QG], PS[po][:, 0:QG], rz[:, 0:QG], ALU.mult, [RP[po], r_rz], [ro])
                                dma(SP, sq["ymixT"][1024 + h * 128:1024 + (h + 1) * 128, q0:q0 + QG], o_[:, 0:QG], [ro],
                                    [sq["r_ymixT"]])
                            S.barrier()
                S.barrier()
                ckpt(f"p2c_{l}")

            is_moe = (l % 2 == 1)
            dff = DEXP if is_moe else DFF
            FS = 256
            nseg = dff // FS
            for g, tiles in enumerate(groups):
                r = seqs[tiles[0][0]]["r"]
                ntile = len(tiles)
                ntok = ntile * 128
                with contextlib.ExitStack() as esg:
                    def sbg(name, shape, dt):
                        return esg.enter_context(nc.sbuf_tensor(f"{name}_{l}_{g}", list(shape), dt))
                    h2T = sbg("h2T", [128, KC, 1024], BF16)
                    r_h2T = Res()
                    gates = sbg("gates", [128, 8, NE], F32)
                    r_gates = Res()
                    with contextlib.ExitStack() as es:
                        def sb(name, shape, dt):
                            return es.enter_context(nc.sbuf_tensor(f"{name}_{l}_{g}", list(shape), dt))
                        ymT = sb("ymT", [128, 16, 1024], BF16)
                        r_ymT = Res()
                        wo = sb("wo", [128, 16, D], BF16)
                        r_wo = Res()
                        G1 = sb("G1", [128, D], F32)
                        r_G1 = Res()
                        xts = [sb(f"x3t{i}", [128, D], F32) for i in range(2)]
                        r_xts = [Res(), Res()]
                        yv = sb("yv", [128, D], F32)
                        r_yv = Res()
                        xn = sb("x3n", [128, D], F32)
                        r_xn = Res()
                        sqs = sb("x3sq", [128, D], F32)
                        r_sq = Res()
                        ssc = sb("x3ss", [128, 16], F32)
                        r_ss = Res()
                        hTf = sb("hTf", [128, KC, 128], F32)
                        r_hTf = Res()
                        wr = sb("wr", [128, KC, NE], BF16)
                        brt = sb("brt", [128, NE], F32)
                        r_wr = Res()
                        lgt = sb("lgt", [128, 4, NE], F32)
                        r_lgt = Res()
                        for kk in range(0, 16, 4):
                            dma(POOL, wo[:, kk:kk + 4, :], w_out[l][kk * 128:(kk + 4) * 128, :].rearrange("(k p) n -> p k n", p=128),
                                [], [r_wo])
                        dma(SP, G1[:], GD[l][0][r].partition_broadcast(128), [r_GD], [r_G1])
                        if is_moe:
                            dma(POOL, wr[:], w_rt.rearrange("(k p) n -> p k n", p=128), [], [r_wr])
                            dma(SP, brt[:], b_rt.partition_broadcast(128), [], [r_wr])
                        for (s, t0, nt, col) in runs(tiles):
                            dma(SP, ymT[:, :, col:col + nt * 128],
                                seqs[s]["ymixT"][:, t0 * 128:(t0 + nt) * 128].rearrange("(k p) t -> p k t", p=128),
                                [seqs[s]["r_ymixT"]], [r_ymT])
                        for ti, (s, t) in enumerate(tiles):
                            sq = seqs[s]
                            xt, r_xt = xts[ti % 2], r_xts[ti % 2]
                            src = sq["xin"] if l == 0 else sq["xr"]
                            dma(SP, xt[:], src[t * 128:(t + 1) * 128, :], [sq["r_xr"]] if l else [], [r_xt])
                            for dq in range(0, D, 512):
                                nq_ = min(512, D - dq)
                                pb = dq // 512
                                for k in range(16):
                                    mm(PS[pb][:, 0:nq_], ymT[:, k, ti * 128:(ti + 1) * 128], wo[:, k, dq:dq + nq_], k == 0, k == 15,
                                       [r_ymT, r_wo], [RP[pb]])
                                act(yv[:, dq:dq + nq_], PS[pb][:, 0:nq_], AF.Copy, [RP[pb]], [r_yv])
                            act(sqs[:], yv[:], AF.Square, [r_yv], [r_sq, r_ss], accum_out=ssc[:, 2:3])
                            rstd_from_sum(ssc[:, 2:3], ssc[:, 2:3], D, [r_ss], [r_ss])
                            stt(yv[:], yv[:], ssc[:, 2:3], G1[:], ALU.mult, ALU.mult, [r_yv, r_ss, r_G1], [r_yv])
                            tt(POOL, xt[:], xt[:], yv[:], ALU.add, [r_xt, r_yv], [r_xt])
                            dma(SP, sq["xr"][t * 128:(t + 1) * 128, :], xt[:], [r_xt], [sq["r_xr"]])
                            norm_transpose(xt[:], r_xt, xn[:], r_xn, ssc[:, 0:1], r_ss, sqs[:], r_sq, h2T, r_h2T, ti * 128, 1, r,
                                           [4, 5])
                            if is_moe:
                                for k in range(KC):
                                    mm(PS[6][:, 0:NE], h2T[:, k, ti * 128:(ti + 1) * 128], wr[:, k, :], k == 0, k == KC - 1,
                                       [r_h2T, r_wr], [RP[6]])
                                tt(DVE, lgt[:, 0, :], PS[6][:, 0:NE], brt[:], ALU.add, [RP[6], r_wr], [r_lgt])
                                rmax(ssc[:, 4:5], lgt[:, 0, :], [r_lgt], [r_ss])
                                ts(DVE, lgt[:, 1, :], lgt[:, 0, :], ssc[:, 4:5], None, ALU.is_equal, None, [r_lgt, r_ss], [r_lgt])
                                stt(lgt[:, 2, :], lgt[:, 1, :], -1e30, lgt[:, 0, :], ALU.mult, ALU.add, [r_lgt], [r_lgt])
                                rmax(ssc[:, 5:6], lgt[:, 2, :], [r_lgt], [r_ss])
                                ts(DVE, lgt[:, 3, :], lgt[:, 2, :], ssc[:, 5:6], None, ALU.is_equal, None, [r_lgt, r_ss], [r_lgt])
                                tt(DVE, ssc[:, 6:7], ssc[:, 5:6], ssc[:, 4:5], ALU.subtract, [r_ss], [r_ss])
                                act(ssc[:, 6:7], ssc[:, 6:7], AF.Exp, [r_ss], [r_ss])
                                ts(DVE, ssc[:, 6:7], ssc[:, 6:7], 1.0, None, ALU.add, None, [r_ss], [r_ss])
                                recip(ssc[:, 7:8], ssc[:, 6:7], [r_ss], [r_ss])
                                ts(DVE, ssc[:, 8:9], ssc[:, 7:8], -1.0, 1.0, ALU.mult, ALU.add, [r_ss], [r_ss])
                                ts(DVE, lgt[:, 1, :], lgt[:, 1, :], ssc[:, 7:8], None, ALU.mult, None, [r_lgt, r_ss], [r_lgt])
                                stt(gates[:, ti, :], lgt[:, 3, :], ssc[:, 8:9], lgt[:, 1, :], ALU.mult, ALU.add, [r_lgt, r_ss],
                                    [r_gates])
                        S.barrier()
                        ckpt(f"p3_{l}")
                        ckpt(f"p3g{g}_{l}")
                    with contextlib.ExitStack() as es:
                        def sb(name, shape, dt):
                            return es.enter_context(nc.sbuf_tensor(f"{name}_{l}_{g}", list(shape), dt))
                        yacc = sb("yacc", [128, 8, D], F32)
                        r_yacc = [Res() for _ in range(8)]
                        wgs = [sb(f"wg{i}", [128, KC, FS], BF16) for i in range(2)]
                        wus = [sb(f"wu{i}", [128, KC, FS], BF16) for i in range(2)]
                        wds = [sb(f"wd{i}", [128, FS // 128, D], BF16) for i in range(2)]
                        r_wg, r_wu, r_wd = [Res(), Res()], [Res(), Res()], [Res(), Res()]
                        hid = [sb(f"hid{i}", [128, FS // 128, 1024], BF16) for i in range(2)]
                        r_hid = [Res(), Res()]
                        sgm = [sb(f"sgm{i}", [128, 512], F32) for i in range(2)]
                        r_sgm = [Res(), Res()]
                        G2 = sb("G2", [128, D], F32)
                        r_G2 = Res()
                        xt4 = [sb(f"x4t{i}", [128, D], F32) for i in range(2)]
                        r_xt4 = [Res(), Res()]
                        sq4 = sb("x4sq", [128, D], F32)
                        ss4 = sb("x4ss", [128, 4], F32)
                        r_sq4, r_ss4 = Res(), Res()
                        dma(SP, G2[:], GD[l][1][r].partition_broadcast(128), [r_GD], [r_G2])
                        nfc = FS // 128
                        halves = [(i, min(512, ntok - i)) for i in range(0, ntok, 512)]
                        segi = 0
                        for e_ in range(NE if is_moe else 1):
                            Wg = moe_g[e_] if is_moe else ffn_g
                            Wu = moe_u[e_] if is_moe else ffn_u
                            Wd = moe_d[e_] if is_moe else ffn_d
                            for sg_ in range(nseg):
                                b = segi % 2
                                first = (segi == 0)
                                segi += 1
                                f0 = sg_ * FS
                                dma(POOL, wgs[b][:], Wg[:, f0:f0 + FS].rearrange("(k p) n -> p k n", p=128), [], [r_wg[b]])
                                dma(POOL, wus[b][:], Wu[:, f0:f0 + FS].rearrange("(k p) n -> p k n", p=128), [], [r_wu[b]])
                                dma(POOL, wds[b][:], Wd[f0:f0 + FS, :].rearrange("(k p) n -> p k n", p=128), [], [r_wd[b]])
                                for fc in range(nfc):
                                    for hi, (c0, n) in enumerate(halves):
                                        pg, pu = (0, 1) if (fc * 2 + hi) % 2 == 0 else (2, 3)
                                        for k in range(KC):
                                            mm(PS[pg][:, 0:n], wgs[b][:, k, fc * 128:(fc + 1) * 128], h2T[:, k, c0:c0 + n], k == 0,
                                               k == KC - 1, [r_wg[b], r_h2T], [RP[pg]])
                                        for k in range(KC):
                                            mm(PS[pu][:, 0:n], wus[b][:, k, fc * 128:(fc + 1) * 128], h2T[:, k, c0:c0 + n], k == 0,
                                               k == KC - 1, [r_wu[b], r_h2T], [RP[pu]])
                                        sm, rsm = sgm[(fc * 2 + hi) % 2], r_sgm[(fc * 2 + hi) % 2]
                                        act(sm[:, 0:n], PS[pg][:, 0:n], AF.Silu, [RP[pg]], [rsm])
                                        tt(DVE, hid[b][:, fc, c0:c0 + n], PS[pu][:, 0:n], sm[:, 0:n], ALU.mult, [RP[pu], rsm],
                                           [r_hid[b]])
                                for ti in range(ntile):
                                    for dq in range(0, D, 512):
                                        nq_ = min(512, D - dq)
                                        pb = 4 + ((ti * (D // 512 if D >= 512 else 1) + dq // 512) % 4)
                                        for fc in range(nfc):
                                            mm(PS[pb][:, 0:nq_], hid[b][:, fc, ti * 128:(ti + 1) * 128], wds[b][:, fc, dq:dq + nq_],
                                               fc == 0, fc == nfc - 1, [r_hid[b], r_wd[b]], [RP[pb]])
                                        ya = yacc[:, ti, dq:dq + nq_]
                                        if is_moe:
                                            if first:
                                                ts(DVE, ya, PS[pb][:, 0:nq_], gates[:, ti, e_:e_ + 1], None, ALU.mult, None,
                                                   [RP[pb], r_gates], [r_yacc[ti]])
                                            else:
                                                stt(ya, PS[pb][:, 0:nq_], gates[:, ti, e_:e_ + 1], ya, ALU.mult, ALU.add,
                                                    [RP[pb], r_gates, r_yacc[ti]], [r_yacc[ti]])
                                        else:
                                            if first:
                                                cp(DVE, ya, PS[pb][:, 0:nq_], [RP[pb]], [r_yacc[ti]])
                                            else:
                                                tt(DVE, ya, PS[pb][:, 0:nq_], ya, ALU.add, [RP[pb], r_yacc[ti]], [r_yacc[ti]])
                        for ti, (s, t) in enumerate(tiles):
                            sq = seqs[s]
                            xt, r_xt = xt4[ti % 2], r_xt4[ti % 2]
                            dma(SP, xt[:], sq["xr"][t * 128:(t + 1) * 128, :], [sq["r_xr"]], [r_xt])
                            act(sq4[:], yacc[:, ti, :], AF.Square, [r_yacc[ti]], [r_sq4, r_ss4], accum_out=ss4[:, 0:1])
                            rstd_from_sum(ss4[:, 0:1], ss4[:, 0:1], D, [r_ss4], [r_ss4])
                            stt(yacc[:, ti, :], yacc[:, ti, :], ss4[:, 0:1], G2[:], ALU.mult, ALU.mult, [r_yacc[ti], r_ss4, r_G2],
                                [r_yacc[ti]])
                            tt(POOL, xt[:], xt[:], yacc[:, ti, :], ALU.add, [r_xt, r_yacc[ti]], [r_xt])
                            if last:
                                dma(SP, sq["yout"][t * 128:(t + 1) * 128, :], xt[:], [r_xt], [Res()])
                            else:
                                dma(SP, sq["xr"][t * 128:(t + 1) * 128, :], xt[:], [r_xt], [sq["r_xr"]])
                        S.barrier()
                        ckpt(f"p4_{l}")
                        ckpt(f"p4g{g}_{l}")
          except _Stop:
            break
        S.emit_all()
        if cfg.get("verbose"):
            print("signal counts", S.sigcount, {q: len([o for o in S.streams[q] if o.is_dma]) for q in S.n_dma_sems})
    return nc


def _fm(v):
    v = np.asarray(v, np.float32)
    n = v.shape[-1] // 128
    lead = v.shape[:-1]
    a = v.reshape(lead + (n, 128))
    a = np.moveaxis(a, -1, 0)
    return np.ascontiguousarray(a.reshape(128, -1))


def consts(cfg):
    TS, GW = cfg["TS"], cfg["GW"]
    axis = 32
    inv = np.power(10000.0, -np.arange(0, axis, 2, dtype=np.float32) / axis).astype(np.float32)
    t = np.arange(TS)
    row = (t // GW).astype(np.float32)
    col = (t % GW).astype(np.float32)
    ar = (row[:, None] * inv).astype(np.float32)
    ac = (col[:, None] * inv).astype(np.float32)
    ang = np.concatenate([ar, ar, ac, ac], axis=1).T
    c_cos = np.cos(ang).astype(np.float32)
    c_sin = np.sin(ang).astype(np.float32)
    j = np.arange(128)[:, None].astype(np.float32)
    i = np.arange(128)[None, :].astype(np.float32)
    dF = np.maximum(i - j, 0.0)
    trilF = (i >= j).astype(np.float32)
    dB = np.maximum(j - i, 0.0)
    trilB = (j >= i).astype(np.float32)
    idxF = np.broadcast_to(i + 1.0, (128, 128))
    idxB = np.broadcast_to(128.0 - i, (128, 128))
    kcolF = 127.0 - j
    kcolB = j + 0.0
    c_ret = np.concatenate([dF, trilF, dB, trilB, idxF, idxB, kcolF, kcolB], axis=1).astype(np.float32)
    return dict(c_ident=np.eye(128, dtype=np.float32), c_cos=np.ascontiguousarray(c_cos),
                c_sin=np.ascontiguousarray(c_sin), c_ret=np.ascontiguousarray(c_ret))


def make_in_maps(cfg, inputs, n_cores):
    f = lambda a: np.ascontiguousarray(np.asarray(a, np.float32))
    D = cfg["D"]
    KC = D // 128
    cst = consts(cfg)
    shared = dict(
        w_mod=f(inputs["w_mod"]), b_mod=f(inputs["b_mod"]), norm_g=f(inputs["norm_gains"]),
        w_in=f(inputs["w_in"]), w_out=f(inputs["w_out"]),
        ret_dec=f(np.asarray(inputs["ret_decay_logit"]).reshape(L, 8)), ret_gn=f(inputs["ret_gn_g"]),
        w_uq=f(inputs["mla_w_uq"]), kv_norm=f(inputs["mla_kv_norm"]), w_ukv=f(inputs["mla_w_ukv"]),
        ffn_g=f(inputs["ffn_w_gate"][0]), ffn_u=f(inputs["ffn_w_up"][0]), ffn_d=f(inputs["ffn_w_down"][0]),
        w_rt=f(inputs["moe_w_router"][0]), b_rt=f(inputs["moe_b_router"][0]),
        moe_g=f(inputs["moe_w_gate"][0]), moe_u=f(inputs["moe_w_up"][0]), moe_d=f(inputs["moe_w_down"][0]),
    )
    shared["bmodT"] = np.stack([_fm(np.asarray(inputs["b_mod"])[l]) for l in range(L)])
    shared["normT"] = np.stack([_fm(np.asarray(inputs["norm_gains"])[l]) for l in range(L)])
    shared["convwT"] = np.stack([
        np.ascontiguousarray(np.asarray(inputs["conv_w"], np.float32)[l].T.reshape(4, 128, 31).transpose(1, 0, 2).reshape(128, 124))
        for l in range(L)])
    shared["convpT"] = np.stack([
        np.concatenate([_fm(np.asarray(inputs[k])[l]) for k in ("conv_b", "conv_ln_g", "conv_ln_b")], axis=1)
        for l in range(L)])
    shared["qnormT"] = np.stack([_fm(np.asarray(inputs["mla_q_norm"])[l]) for l in range(L)])
    shared["kvnormT"] = np.stack([_fm(np.asarray(inputs["mla_kv_norm"])[l]) for l in range(L)])
    shared.update(cst)
    xs = np.asarray(inputs["x_sample"], np.float32)
    xp = np.asarray(inputs["x_prompt"], np.float32)
    c = np.asarray(inputs["c"], np.float32)
    cc = np.asarray(inputs["c_ctx"], np.float32)
    maps = []
    for core in range(n_cores):
        b = core // 2
        cond = np.stack([c[b], cc])
        condT = np.ascontiguousarray(cond.reshape(2, KC, 128).transpose(2, 1, 0).reshape(128, KC * 2))
        m = dict(shared)
        m.update(x_s=f(xs[b]), x_p=f(xp[core * NPR:(core + 1) * NPR]),
                 ckv_c=f(inputs["cache_mla_ckv"][b]), kpe_c=f(inputs["cache_mla_kpe"][b]),
                 sret=f(inputs["state_ret"][b]), condT=condT)
        maps.append(m)
    return maps


_NC_CACHE = {}


def run(cfg, inputs, n_cores=8):
    key = tuple(sorted(cfg.items()))
    if key not in _NC_CACHE:
        _NC_CACHE[key] = build(cfg)
    nc = _NC_CACHE[key]
    maps = make_in_maps(cfg, inputs, n_cores)
    res = run_bass_kernel_spmd(nc, maps, core_ids=list(range(n_cores)))
    rs = res.results
    if cfg.get("debug_out"):
        return rs
    y_prompt = np.concatenate([rs[i]["y_p"] for i in range(n_cores)], axis=0)
    y_sample = np.stack([rs[2 * b]["y_s"] for b in range(n_cores // 2)], axis=0)
    o_ckv = np.concatenate([rs[i]["o_ckv"] for i in range(n_cores)], axis=0)
    o_kpe = np.concatenate([rs[i]["o_kpe"] for i in range(n_cores)], axis=0)
    o_sret = np.concatenate([rs[i]["o_sret"] for i in range(n_cores)], axis=0)
    return tuple(np.ascontiguousarray(a.astype(np.float32)) for a in (y_prompt, y_sample, o_ckv, o_kpe, o_sret))


def kernel(**inputs):
    return run(FULL_CFG, inputs, 8)
```
